# Optimizing a Trainium2 kernel written in Bass

```python
import math
import jax, jax.numpy as jnp
from jax import lax
import numpy as np

D_MODEL = 1024
BATCH = 8
SEQ = 2048
DEPTH = 2

CHUNK = 64
MEM_LEN = 256
Q_BLOCK = 128
RMS_EPS = 1e-6

DA_HEADS = 8
DA_QK_DIM = 64
DA_V_DIM = 2 * DA_QK_DIM
DA_QK_WIDTH = DA_HEADS * 2 * DA_QK_DIM
DA_WIDTH = DA_HEADS * DA_V_DIM

POOL_WINDOWS = (2, 4, 8, 16)
POOL_GROUPS = 4
POOL_GROUP_DIM = 128
POOL_WIDTH = POOL_GROUPS * POOL_GROUP_DIM

MEM_HEADS = 4
MEM_HEAD_DIM = 128
MEM_WIDTH = MEM_HEADS * MEM_HEAD_DIM

N_BRANCHES = 3
IN_WIDTH = 2 * DA_QK_WIDTH + DA_WIDTH + POOL_WIDTH + MEM_WIDTH

D_FF_DENSE = 2816
N_EXPERTS = 8
TOP_K = 2
D_FF_EXPERT = 3584
N_DENSE = (DEPTH + 1) // 2
N_MOE = DEPTH // 2

kernel_name = "hybrid_diffattn_pool_memxattn_moe"


def rmsnorm(x, g):
    xf = x.astype(jnp.float32)
    xf = xf * lax.rsqrt(jnp.mean(xf * xf, axis=-1, keepdims=True) + RMS_EPS)
    return xf.astype(x.dtype) * g


def alibi_slopes(n_heads):
    return np.array([2.0 ** (-8.0 * (h + 1) / n_heads) for h in range(n_heads)], dtype=np.float32)


def lambda_init(layer):
    return 0.8 - 0.6 * math.exp(-0.3 * layer)


def diff_attention(q, k, v, lam):
    S = q.shape[1]
    scale = DA_QK_DIM ** -0.5
    slopes = jnp.asarray(alibi_slopes(DA_HEADS))[:, None, None, None]
    outs = []
    for i in range(S // Q_BLOCK):
        q0 = i * Q_BLOCK
        kv_len = q0 + Q_BLOCK
        qb = q[:, q0:kv_len]
        kb = k[:, :kv_len]
        vb = v[:, :kv_len]
        s = jnp.einsum('bqhmd,bkhmd->bhmqk', qb, kb,
                       preferred_element_type=jnp.float32) * scale
        q_pos = jnp.arange(q0, kv_len)
        k_pos = jnp.arange(kv_len)
        allowed = (k_pos // CHUNK)[None, :] <= (q_pos // CHUNK)[:, None]
        dist = jnp.abs(q_pos[:, None] - k_pos[None, :]).astype(jnp.float32)
        s = jnp.where(allowed, s - slopes * dist, -1e30)
        p = jax.nn.softmax(s, axis=-1)
        attn = p[:, :, 0] - lam.astype(jnp.float32) * p[:, :, 1]
        outs.append(jnp.einsum('bhqk,bkhd->bqhd', attn.astype(vb.dtype), vb))
    return jnp.concatenate(outs, axis=1)


def pool_mixer(p, w_grp, scale):
    B, S, _ = p.shape
    pg = p.reshape(B, S, POOL_GROUPS, POOL_GROUP_DIM).astype(jnp.float32)
    cum = lax.cumsum(pg, axis=1)
    cum_pad = jnp.concatenate([jnp.zeros_like(cum[:, :1]), cum], axis=1)
    t = jnp.arange(S)
    pooled = []
    for g, w in enumerate(POOL_WINDOWS):
        lo = jnp.maximum(t + 1 - w, 0)
        cnt = (t + 1 - lo).astype(jnp.float32)[None, :, None]
        pooled.append((cum[:, :, g] - cum_pad[:, lo, g]) / cnt)
    pooled = jnp.stack(pooled, axis=2)
    d = (pooled - pg).astype(p.dtype)
    y = jnp.einsum('bsgc,gcd->bsgd', d, w_grp)
    return y.reshape(B, S, POOL_WIDTH) * scale


def mem_attention(q, mem_h, w_mem_kv, g_q, g_k):
    B, S, _ = q.shape
    M = mem_h.shape[1]
    kv = mem_h @ w_mem_kv
    k = rmsnorm(kv[..., :MEM_WIDTH].reshape(B, M, MEM_HEADS, MEM_HEAD_DIM), g_k)
    v = kv[..., MEM_WIDTH:].reshape(B, M, MEM_HEADS, MEM_HEAD_DIM)
    q = rmsnorm(q.reshape(B, S, MEM_HEADS, MEM_HEAD_DIM), g_q)
    s = jnp.einsum('bqhd,bkhd->bhqk', q, k,
                   preferred_element_type=jnp.float32) * (MEM_HEAD_DIM ** -0.5)
    p = jax.nn.softmax(s, axis=-1).astype(v.dtype)
    return jnp.einsum('bhqk,bkhd->bqhd', p, v).reshape(B, S, MEM_WIDTH)


def swiglu(h, w1, w3, w2):
    return (jax.nn.silu(h @ w1) * (h @ w3)) @ w2


def moe_swiglu(h, w_router, w1, w3, w2):
    B, S, D = h.shape
    ht = h.reshape(B * S, D)
    logits = (ht @ w_router).astype(jnp.float32)
    vals, idx = lax.top_k(logits, TOP_K)
    gates = jax.nn.softmax(vals, axis=-1).astype(h.dtype)
    comb = jnp.einsum('tk,tke->te', gates, jax.nn.one_hot(idx, N_EXPERTS, dtype=h.dtype))
    out = jnp.zeros_like(ht)
    for e in range(N_EXPERTS):
        out = out + comb[:, e:e + 1] * swiglu(ht, w1[e], w3[e], w2[e])
    return out.reshape(B, S, D)


def setup_inputs(seed: int = 0) -> dict:
    key = jax.random.key(seed)
    ks = iter(jax.random.split(key, 40))

    def nrm(shape, scale):
        return jax.random.normal(next(ks), shape, jnp.float32) * scale

    def gain(shape):
        return 1.0 + nrm(shape, 0.1)

    D = D_MODEL
    return {
        "x": nrm((BATCH, SEQ, D), 1.0),
        "mem": nrm((BATCH, MEM_LEN, D), 1.0),
        "norm_mix": gain((DEPTH, D)),
        "norm_mem": gain((DEPTH, D)),
        "norm_ffn": gain((DEPTH, D)),
        "w_in": nrm((DEPTH, D, IN_WIDTH), D ** -0.5),
        "w_gate": nrm((DEPTH, D, N_BRANCHES * D), D ** -0.5),
        "b_gate": nrm((DEPTH, N_BRANCHES * D), 0.1),
        "da_q_norm": gain((DEPTH, 2, DA_QK_DIM)),
        "da_k_norm": gain((DEPTH, 2, DA_QK_DIM)),
        "da_lam_q1": nrm((DEPTH, DA_QK_DIM), 0.1),
        "da_lam_k1": nrm((DEPTH, DA_QK_DIM), 0.1),
        "da_lam_q2": nrm((DEPTH, DA_QK_DIM), 0.1),
        "da_lam_k2": nrm((DEPTH, DA_QK_DIM), 0.1),
        "da_out_norm": gain((DEPTH, DA_V_DIM)),
        "pool_w": nrm((DEPTH, POOL_GROUPS, POOL_GROUP_DIM, POOL_GROUP_DIM), POOL_GROUP_DIM ** -0.5),
        "pool_scale": gain((DEPTH, POOL_WIDTH)),
        "mem_q_norm": gain((DEPTH, MEM_HEAD_DIM)),
        "mem_k_norm": gain((DEPTH, MEM_HEAD_DIM)),
        "w_mem_kv": nrm((DEPTH, D, 2 * MEM_WIDTH), D ** -0.5),
        "w_br_da": nrm((DEPTH, DA_WIDTH, D), DA_WIDTH ** -0.5),
        "w_br_pool": nrm((DEPTH, POOL_WIDTH, D), POOL_WIDTH ** -0.5),
        "w_br_mem": nrm((DEPTH, MEM_WIDTH, D), MEM_WIDTH ** -0.5),
        "w_out": nrm((DEPTH, D, D), D ** -0.5),
        "ffn_w1": nrm((N_DENSE, D, D_FF_DENSE), D ** -0.5),
        "ffn_w3": nrm((N_DENSE, D, D_FF_DENSE), D ** -0.5),
        "ffn_w2": nrm((N_DENSE, D_FF_DENSE, D), D_FF_DENSE ** -0.5),
        "moe_router": nrm((N_MOE, D, N_EXPERTS), D ** -0.5),
        "moe_w1": nrm((N_MOE, N_EXPERTS, D, D_FF_EXPERT), D ** -0.5),
        "moe_w3": nrm((N_MOE, N_EXPERTS, D, D_FF_EXPERT), D ** -0.5),
        "moe_w2": nrm((N_MOE, N_EXPERTS, D_FF_EXPERT, D), D_FF_EXPERT ** -0.5),
    }


def reference(x, mem, norm_mix, norm_mem, norm_ffn, w_in, w_gate, b_gate,
              da_q_norm, da_k_norm, da_lam_q1, da_lam_k1, da_lam_q2, da_lam_k2, da_out_norm,
              pool_w, pool_scale, mem_q_norm, mem_k_norm, w_mem_kv,
              w_br_da, w_br_pool, w_br_mem, w_out,
              ffn_w1, ffn_w3, ffn_w2, moe_router, moe_w1, moe_w3, moe_w2):
    B, S, D = x.shape
    c_q = DA_QK_WIDTH
    c_k = c_q + DA_QK_WIDTH
    c_v = c_k + DA_WIDTH
    c_p = c_v + POOL_WIDTH
    for l in range(DEPTH):
        h = rmsnorm(x, norm_mix[l])
        proj = h @ w_in[l]
        q = rmsnorm(proj[..., :c_q].reshape(B, S, DA_HEADS, 2, DA_QK_DIM), da_q_norm[l])
        k = rmsnorm(proj[..., c_q:c_k].reshape(B, S, DA_HEADS, 2, DA_QK_DIM), da_k_norm[l])
        v = proj[..., c_k:c_v].reshape(B, S, DA_HEADS, DA_V_DIM)
        p_in = proj[..., c_v:c_p]
        mq = proj[..., c_p:]

        lam_0 = lambda_init(l)
        lam = (jnp.exp(jnp.sum(da_lam_q1[l] * da_lam_k1[l]))
               - jnp.exp(jnp.sum(da_lam_q2[l] * da_lam_k2[l])) + lam_0)
        o_da = diff_attention(q, k, v, lam)
        o_da = (rmsnorm(o_da, da_out_norm[l]) * (1.0 - lam_0)).reshape(B, S, DA_WIDTH)

        o_pool = pool_mixer(p_in, pool_w[l], pool_scale[l])
        o_mem = mem_attention(mq, rmsnorm(mem, norm_mem[l]), w_mem_kv[l], mem_q_norm[l], mem_k_norm[l])

        gates = jax.nn.sigmoid(h @ w_gate[l] + b_gate[l]).reshape(B, S, N_BRANCHES, D)
        merged = (gates[:, :, 0] * (o_da @ w_br_da[l])
                  + gates[:, :, 1] * (o_pool @ w_br_pool[l])
                  + gates[:, :, 2] * (o_mem @ w_br_mem[l]))
        x = x + merged @ w_out[l]

        h2 = rmsnorm(x, norm_ffn[l])
        if l % 2 == 0:
            j = l // 2
            x = x + swiglu(h2, ffn_w1[j], ffn_w3[j], ffn_w2[j])
        else:
            j = l // 2
            x = x + moe_swiglu(h2, moe_router[j], moe_w1[j], moe_w3[j], moe_w2[j])
    return x
```

```python
import math
import numpy as np
import concourse.bass as bass
import concourse.mybir as mybir
from concourse.bass_utils import run_bass_kernel_spmd

F32 = mybir.dt.float32
BF16 = mybir.dt.bfloat16
AF = mybir.ActivationFunctionType
ALU = mybir.AluOpType

D = 1024; S = 2048; KC = 8; TB = 4; DEPTH = 2
NH = 8; MEM = 256; MH = 4
DFF = 2816; NE = 8; DFE = 3584
EPS = 1e-6
NSLOT = 8
NPC = 320
NCC = 8 * 20 + 8 * 128 + 16 + 128


GROUPW = (128, 256, 512, 512, 512, 512, 512, 512)


def alibi_slopes():
    return [2.0 ** (-8.0 * (h + 1) / NH) for h in range(NH)]


def lambda_init(layer):
    return 0.8 - 0.6 * math.exp(-0.3 * layer)


class Buf:
    __slots__ = ("w", "r", "rd")

    def __init__(self):
        self.w = None; self.r = {}; self.rd = []


class V:
    __slots__ = ("ap", "bufs")

    def __init__(self, ap, bufs):
        self.ap = ap; self.bufs = bufs

    def __getitem__(self, idx):
        return V(self.ap[idx], self.bufs)


class Op:
    __slots__ = ("eng", "fn", "deps", "sig", "sem", "val", "dma", "prev", "wr")


COMPUTE = ("pe", "act", "dve", "pool")
_DSZ = {F32: 4, BF16: 2}


def _byte_range(ap):
    try:
        sz = _DSZ[ap.dtype]
        dims = list(ap.ap)[1:]
        span = 1
        for st, cnt in dims:
            if st < 0: return None
            span += (cnt - 1) * st
        pstride = list(ap.ap)[0][0]
        off = ap.offset % pstride if pstride else ap.offset
        return (ap.tensor.name, off * sz, (off + span) * sz)
    except Exception:
        return None


class Prog:
    def __init__(self, nc):
        self.nc = nc
        self.ops = {e: [] for e in ("pe", "act", "dve", "pool", "sp")}

    def op(self, eng, fn, reads=(), writes=(), dma=False):
        o = Op(); o.eng = eng; o.fn = fn; o.dma = dma; o.sig = dma; o.sem = None; o.val = 0; o.prev = None
        o.wr = _byte_range(writes[0].ap) if (len(writes) == 1 and isinstance(writes[0], V)) else None
        deps = []
        rb = []; wb = []
        for v in reads: rb.extend(v.bufs)
        for v in writes: wb.extend(v.bufs)
        for b in rb:
            if b.w is not None: deps.append((b.w, 0))
        for b in wb:
            if b.w is not None: deps.append((b.w, 1))
            for r in b.r.values(): deps.append((r, 2))
            for r in b.rd: deps.append((r, 2))
        keep = {}
        for d, kind in deps:
            if d is o: continue
            if d.eng == eng and not dma and not d.dma:
                if eng == "pe": continue
                if kind == 1 and o.wr is not None and d.wr is not None and \
                        (o.wr[0] != d.wr[0] or o.wr[1] >= d.wr[2] or d.wr[1] >= o.wr[2]):
                    continue
            keep[id(d)] = d
        o.deps = list(keep.values())
        for d in o.deps: d.sig = True
        for b in rb:
            if dma: b.rd.append(o)
            else: b.r[eng] = o
        for b in wb:
            b.w = o; b.r = {}; b.rd = []
        self.ops[eng].append(o)
        return o

    def mm(self, out, lhsT, rhs, start=True, stop=True):
        o, l, r = out.ap, lhsT.ap, rhs.ap
        return self.op("pe", lambda e: e.matmul(o, lhsT=l, rhs=r, start=start, stop=stop),
                       [lhsT, rhs], [out])

    def act(self, out, in_, func, bias=None, scale=1.0, eng="act"):
        o, i = out.ap, in_.ap
        rd = [in_]
        if isinstance(bias, V):
            rd.append(bias); b = bias.ap
        else:
            b = 0.0 if bias is None else float(bias)
        if isinstance(scale, V):
            rd.append(scale); s = scale.ap
        else:
            s = float(scale)
        return self.op(eng, lambda e: e.activation(out=o, in_=i, func=func, bias=b, scale=s), rd, [out])

    def stt(self, out, in0, scalar, in1, op0, op1, eng="dve"):
        o, a, c = out.ap, in0.ap, in1.ap
        rd = [in0, in1]
        if isinstance(scalar, V):
            rd.append(scalar); s = scalar.ap
        else:
            s = float(scalar)
        return self.op(eng, lambda e: e.scalar_tensor_tensor(out=o, in0=a, scalar=s, in1=c, op0=op0, op1=op1),
                       rd, [out])

    def tt(self, out, in0, in1, op, eng="dve"):
        o, a, c = out.ap, in0.ap, in1.ap
        return self.op(eng, lambda e: e.tensor_tensor(out=o, in0=a, in1=c, op=op), [in0, in1], [out])

    def ts(self, out, in0, s1, s2, op0, op1=None, eng="dve"):
        o, a = out.ap, in0.ap
        rd = [in0]
        if isinstance(s1, V): rd.append(s1); s1 = s1.ap
        if isinstance(s2, V): rd.append(s2); s2 = s2.ap
        if op1 is None:
            return self.op(eng, lambda e: e.tensor_scalar(out=o, in0=a, scalar1=s1, scalar2=None, op0=op0), rd, [out])
        return self.op(eng, lambda e: e.tensor_scalar(out=o, in0=a, scalar1=s1, scalar2=s2, op0=op0, op1=op1), rd, [out])

    def copy(self, out, in_, eng="dve"):
        o, i = out.ap, in_.ap
        return self.op(eng, lambda e: e.tensor_copy(out=o, in_=i), [in_], [out])

    def recip(self, out, in_):
        o, i = out.ap, in_.ap
        return self.op("dve", lambda e: e.reciprocal(out=o, in_=i), [in_], [out])

    def memset(self, out, val, eng="dve"):
        o = out.ap
        return self.op(eng, lambda e: e.memset(o, val), [], [out])

    def reduce_sum(self, out, in_):
        o, i = out.ap, in_.ap
        return self.op("dve", lambda e: e.reduce_sum(out=o, in_=i, axis=mybir.AxisListType.X), [in_], [out])

    def reduce_max(self, out, in_):
        o, i = out.ap, in_.ap
        return self.op("dve", lambda e: e.reduce_max(out=o, in_=i, axis=mybir.AxisListType.X), [in_], [out])

    def dma(self, q, out, in_, reads=(), writes=()):
        o = out.ap if isinstance(out, V) else out
        i = in_.ap if isinstance(in_, V) else in_
        rd = list(reads); wr = list(writes)
        if isinstance(in_, V): rd.append(in_)
        if isinstance(out, V): wr.append(out)
        return self.op(q, lambda e: e.dma_start(out=o, in_=i), rd, wr, dma=True)

    def emit(self, final_ops):
        nc = self.nc
        from contextlib import ExitStack
        NSD = {"sp": 12, "pool": 12, "act": 2, "dve": 1, "pe": 1}
        with ExitStack() as es:
            esem = {e: es.enter_context(nc.semaphore("s_" + e)) for e in COMPUTE}
            dsem = {q: [es.enter_context(nc.semaphore("d_%s%d" % (q, k))) for k in range(NSD[q])]
                    for q in ("sp", "pool")}
            for e, lst in self.ops.items():
                c = 0; nd = 0; dlist = []
                for o in lst:
                    if o.dma:
                        ns = len(dsem[e])
                        o.sem = dsem[e][nd % ns]; o.val = 16 * (nd // ns + 1)
                        if nd >= ns: o.prev = dlist[nd - ns]
                        dlist.append(o); nd += 1
                    elif o.sig:
                        c += 1; o.sem = esem[e]; o.val = c
            block = es.enter_context(nc.Block())
            prog = self

            def run(engname, e):
                waited = {}
                lst = prog.ops[engname]
                for o in lst:
                    need = {}
                    deps = o.deps if o.prev is None else o.deps + [o.prev]
                    for d in deps:
                        k = id(d.sem)
                        if waited.get(k, 0) >= d.val: continue
                        if k not in need or need[k][1] < d.val: need[k] = (d.sem, d.val)
                    for k, (sem, val) in need.items():
                        e.wait_ge(sem, val); waited[k] = val
                    inst = o.fn(e)
                    if o.sig:
                        inst.then_inc(o.sem, 16 if o.dma else 1)
                if engname == "sp":
                    for o in final_ops:
                        if waited.get(id(o.sem), 0) < o.val:
                            e.wait_ge(o.sem, o.val); waited[id(o.sem)] = o.val

            @block.tensor
            def _(e): run("pe", e)

            @block.scalar
            def _(e): run("act", e)

            @block.vector
            def _(e): run("dve", e)

            @block.gpsimd
            def _(e): run("pool", e)

            @block.sync
            def _(e): run("sp", e)


def build_program(n_layers=DEPTH, stop_after=None, dump=None):
    nc = bass.Bass("TRN2", target_bir_lowering=False)
    P = Prog(nc)
    unit_keys = []
    from contextlib import ExitStack
    es = ExitStack()

    xT = nc.dram_tensor("xT", [D, S], F32, kind="ExternalInput").ap()
    memT = nc.dram_tensor("memT", [D, MEM], F32, kind="ExternalInput").ap()
    params = nc.dram_tensor("params", [128, DEPTH * NPC], F32, kind="ExternalInput").ap()
    consts = nc.dram_tensor("consts", [128, NCC], F32, kind="ExternalInput").ap()
    router = nc.dram_tensor("router", [128, KC * NE], F32, kind="ExternalInput").ap()
    holder = {}
    outT = nc.dram_tensor("outT", [D, S], F32, kind="ExternalOutput").ap()

    def sb(name, shape, dt):
        return es.enter_context(nc.sbuf_tensor(name, shape, dt))

    def ps(name):
        return es.enter_context(nc.psum_tensor(name, [128, 512], F32))

    Xt = sb("X", [128, KC, S], F32)
    Ht = sb("H", [128, KC, S], BF16)
    Ot = sb("O", [128, KC, S], BF16)
    UN = 38 * 512
    Ut = sb("U", [128, UN], BF16)
    Wt = sb("W", [128, NSLOT, 1024], BF16)
    SQt = sb("SQ", [128, 4, 512], BF16)
    LTt = sb("LT", [128, 2, 512], F32)
    RTt = sb("RT", [128, 2, 512], F32)
    PRt = sb("PR", [128, DEPTH * NPC], F32)
    CTt = sb("CT", [128, NCC], F32)
    ONEt = sb("ONES", [128, 128], BF16)
    BDt = sb("BD64", [128, 128], BF16)
    SMt = sb("SM", [128, 64], F32)
    CARt = sb("CAR", [128, 4, 16], F32)
    WPWt = sb("WPW", [128, 512], BF16)
    PSt = [ps("ps%d" % k) for k in range(8)]

    def grid(t, n1, n2, w):
        return [[V(t[:, a, b * w:(b + 1) * w], [Buf()]) for b in range(n2)] for a in range(n1)]

    X = grid(Xt, KC, TB, 512)
    H = grid(Ht, KC, TB, 512)
    O = grid(Ot, KC, TB, 512)
    PS = [V(p[:], [Buf()]) for p in PSt]
    Wslot = [V(Wt[:, k, :], [Buf()]) for k in range(NSLOT)]
    SQ = [V(SQt[:, k, :], [Buf()]) for k in range(4)]
    LT = [V(LTt[:, k, :], [Buf()]) for k in range(2)]
    RT = [V(RTt[:, k, :], [Buf()]) for k in range(2)]
    PR = V(PRt[:], [Buf()])
    CT = V(CTt[:], [Buf()])
    ONES = V(ONEt[:], [Buf()])
    BD64 = V(BDt[:], [Buf()])
    SM = V(SMt[:], [Buf()])
    UG = [Buf() for _ in range(UN // 512)]

    def ubf(off, n, shape=None):
        ap = Ut[:, off:off + n]
        if shape: ap = ap.rearrange(shape[0], **shape[1])
        return V(ap, UG[off // 512:(off + n + 511) // 512])

    def uf32(off, n, shape=None):
        ap = Ut[:, off:off + n].bitcast(F32)
        if shape: ap = ap.rearrange(shape[0], **shape[1])
        return V(ap, UG[off // 512:(off + n + 511) // 512])

    cnt = {"rot": 0, "sq": 0, "lt": 0, "w": 0}

    rotbanks = {"b": (4, 5, 6, 7)}

    def rot():
        bb = rotbanks["b"]
        k = bb[cnt["rot"] % len(bb)]; cnt["rot"] += 1
        return PS[k]

    def wload(key):
        idx = len(unit_keys); unit_keys.append(key)
        slot = Wslot[cnt["w"] % NSLOT]; cnt["w"] += 1
        so = slot.ap
        P.op("pool", lambda e: e.dma_start(out=so, in_=holder["w"][idx]), [], [slot], dma=True)
        return slot

    def pcol(l, c, n=1):
        return PR[:, l * NPC + c: l * NPC + c + n]

    C_GMIX, C_GMEM, C_GFFN, C_BG, C_QG, C_KG, C_OG, C_PSC, C_MQG, C_MKG, C_LAM = 0, 8, 16, 24, 48, 49, 50, 51, 55, 56, 57
    K_BOFF, K_BDIAG, K_INVC, K_ID = 0, 160, 160 + 1024, 160 + 1024 + 16

    P.dma("sp", PR, params)
    P.dma("sp", CT, consts)
    for i in range(KC):
        for tb in range(TB):
            P.dma("sp", X[i][tb], xT[i * 128:(i + 1) * 128, tb * 512:(tb + 1) * 512])
    P.memset(ONES, 1.0)
    P.memset(BD64, 0.0)
    P.memset(BD64[0:64, 0:64], 1.0)
    P.memset(BD64[64:128, 64:128], 1.0)

    def rstd_from(srcs, ones, n, lnbias=0.0, sbuf_src=0):
        N = srcs[0].ap.shape[-1]
        ss = rot()
        for i, s_ in enumerate(srcs):
            sq = SQ[cnt["sq"] % 4]; cnt["sq"] += 1
            if sbuf_src == 2 or (sbuf_src == 1 and i % 2 == 1):
                P.tt(sq[:, 0:N], s_, s_, ALU.mult)
            else:
                P.act(sq[:, 0:N], s_, AF.Square)
            P.mm(ss[:, 0:N], ones, sq[:, 0:N], start=(i == 0), stop=(i == len(srcs) - 1))
        k = cnt["lt"] % 2; cnt["lt"] += 1
        P.act(RT[k][:, 0:N], ss[:, 0:N], AF.Ln, bias=EPS, scale=1.0 / n)
        P.act(RT[k][:, 0:N], RT[k][:, 0:N], AF.Exp, bias=lnbias, scale=-0.5)
        return RT[k][:, 0:N]

    def rmsnorm_to_H(l, gcol):
        for tb in range(TB):
            R = rstd_from([X[i][tb] for i in range(KC)], ONES, D, sbuf_src=1)
            for i in range(KC):
                P.stt(H[i][tb], X[i][tb], pcol(l, gcol + i), R, ALU.mult, ALU.mult)

    def proj_fm(w, rhs_list, out_ps, kcs=None):
        n = len(rhs_list)
        for kc in range(n):
            P.mm(out_ps, w[:, kc * 128:(kc + 1) * 128], rhs_list[kc], start=(kc == 0), stop=(kc == n - 1))

    final_ops = []

    def dump_out(name, views):
        for j, v in enumerate(views):
            n = v.ap.shape[-1]
            t = nc.dram_tensor("%s_%d" % (name, j), [128, n], F32, kind="ExternalOutput").ap()
            final_ops.append(P.dma("pool", t, v))

    slopes = alibi_slopes()

    for l in range(n_layers):
        lam0 = lambda_init(l)
        lv = [pcol(l, C_LAM + 64 * j, 64) for j in range(4)]
        tmp = uf32(0, 128)
        P.tt(tmp, lv[0], lv[1], ALU.mult)
        P.reduce_sum(SM[:, 1:2], tmp)
        P.tt(tmp, lv[2], lv[3], ALU.mult)
        P.reduce_sum(SM[:, 2:3], tmp)
        P.act(SM[:, 3:5], SM[:, 1:3], AF.Exp)
        P.tt(SM[:, 5:6], SM[:, 4:5], SM[:, 3:4], ALU.subtract)
        P.act(SM[:, 0:1], SM[:, 5:6], AF.Identity, bias=-lam0)

        rmsnorm_to_H(l, C_GMIX)
        if stop_after == "norm1" and l == n_layers - 1:
            break

        QN = [[ubf((s_ * 4 + tb) * 512, 512) for tb in range(TB)] for s_ in range(2)]
        KN = [[ubf(4096 + (s_ * 4 + tb) * 512, 512) for tb in range(TB)] for s_ in range(2)]
        VH = [[ubf(8192 + (s_ * 4 + tb) * 512, 512, ("p (a b) -> p a b", dict(a=4))) for tb in range(TB)]
              for s_ in range(2)]
        PT = [ubf(12288 + s_ * 1024, 1024, ("p (a b) -> p a b", dict(a=2))) for s_ in range(2)]
        TMP = [uf32(14336 + s_ * 512, 256) for s_ in range(2)]
        FR = [uf32(15360 + k * 1024, 1024) for k in range(4)]
        def proj_chunks(h):
            st = h % 2
            box = {}
            chunks = []

            def qk_chunk(which, tb):
                def f():
                    if tb == 0:
                        box[which] = wload(("win", l, (0 if which == "q" else 8) + h))
                    pq = rot()
                    proj_fm(box[which], [H[kc][tb] for kc in range(KC)], pq)
                    R = rstd_from([pq], BD64, 64)
                    dst = QN[st][tb] if which == "q" else KN[st][tb]
                    P.stt(dst, pq, pcol(l, C_QG if which == "q" else C_KG), R, ALU.mult, ALU.mult)
                return f

            def v_chunk(tb):
                def f():
                    if tb == 0:
                        box["v"] = wload(("win", l, 16 + h))
                    wv = box["v"]
                    pv = rot()
                    for j in range(4):
                        for kc in range(KC):
                            P.mm(pv[:, j * 128:(j + 1) * 128], H[kc][tb][:, j * 128:(j + 1) * 128],
                                 wv[:, kc * 128:(kc + 1) * 128], start=(kc == 0), stop=(kc == KC - 1))
                    P.copy(VH[st][tb], V(pv.ap.rearrange("p (a b) -> p a b", a=4), pv.bufs))
                return f
            for tb in range(TB): chunks.append(qk_chunk("q", tb))
            for tb in range(TB): chunks.append(qk_chunk("k", tb))
            for tb in range(TB): chunks.append(v_chunk(tb))
            return chunks

        for c in proj_chunks(0): c()
        for h in range(NH):
            st = h % 2
            nxt = proj_chunks(h + 1) if h + 1 < NH else []
            iters = [(G, kb) for G in range(TB) for kb in range(4 * G + 4)]
            ACC = [PS[0], PS[1]]; SUM = [PS[2], PS[3]]

            def emit_S(G, kb, par):
                j0 = max(0, kb - 4 * G); c0 = j0 * 128
                Sm = [PS[4 + 2 * par], PS[5 + 2 * par]]
                kt = KN[st][kb // 4]; kc0 = (kb % 4) * 128
                for m in range(2):
                    P.mm(Sm[m][:, c0:512], kt[64 * m:64 * m + 64, kc0:kc0 + 128],
                         QN[st][G][64 * m:64 * m + 64, c0:512])
                return Sm
            rotbanks["b"] = (6, 7)
            S_next = emit_S(*iters[0], 0)
            for n, (G, kb) in enumerate(iters):
                Sm = S_next
                if n + 1 < len(iters): S_next = emit_S(*iters[n + 1], (n + 1) % 2)
                rotbanks["b"] = (4 + 2 * (n % 2), 5 + 2 * (n % 2))
                nkb = 4 * G + 4
                j0 = max(0, kb - 4 * G); c0 = j0 * 128
                Pm = PT[n % 2]
                Wh = GROUPW[h]; nbk = Wh // 128
                for m in range(2):
                    for u in range(512 // Wh):
                        jb0 = u * nbk; jb1 = jb0 + nbk
                        if jb1 <= j0: continue
                        if kb < 4 * G + jb0:
                            dd = 4 * G + jb0 - kb
                            cs = slice(jb0 * 128, jb1 * 128)
                            P.act(Pm[:, m, cs], Sm[m][:, cs], AF.Exp,
                                  bias=CT[:, K_BOFF + h * 20 + dd + 3:K_BOFF + h * 20 + dd + 4], scale=0.125)
                        else:
                            jk = kb - 4 * G; jl = jk - jb0
                            cs = slice(jk * 128, (jk + 1) * 128)
                            t_ = TMP[m]
                            P.stt(t_, Sm[m][:, cs], 0.125, CT[:, K_BDIAG + h * 128:K_BDIAG + (h + 1) * 128],
                                  ALU.mult, ALU.add)
                            P.act(Pm[:, m, cs], t_, AF.Exp, bias=slopes[h] * (128.0 * jl - Wh / 2.0 + 64.0))
                            if jk + 1 < jb1:
                                cs = slice((jk + 1) * 128, jb1 * 128)
                                P.act(Pm[:, m, cs], Sm[m][:, cs], AF.Exp,
                                      bias=CT[:, K_BOFF + h * 20 - jl + 3:K_BOFF + h * 20 - jl + 4], scale=0.125)
                for m in range(2):
                    P.mm(ACC[m][:, c0:512], VH[st][kb // 4][:, kb % 4, :], Pm[:, m, c0:512],
                         start=(kb == 0), stop=(kb == nkb - 1))
                    P.mm(SUM[m][:, c0:512], ONES, Pm[:, m, c0:512], start=(kb == 0), stop=(kb == nkb - 1))
                if n % 3 == 1 and nxt:
                    nxt.pop(0)()
                if kb == nkb - 1:
                    P.recip(FR[0], SUM[0]); P.recip(FR[1], SUM[1])
                    P.tt(FR[2], ACC[0], FR[0], ALU.mult)
                    P.tt(FR[3], ACC[1], FR[1], ALU.mult)
                    P.stt(FR[0], FR[3], SM[:, 0:1], FR[2], ALU.mult, ALU.add)
                    R = rstd_from([FR[0]], ONES, 128, lnbias=math.log(1.0 - lam0), sbuf_src=2)
                    P.stt(O[h][G], FR[0], pcol(l, C_OG), R, ALU.mult, ALU.mult)
            rotbanks["b"] = (4, 5, 6, 7)
            while nxt:
                nxt.pop(0)()
        if stop_after == "attn" and l == n_layers - 1:
            break

        MT = uf32(0, 4096, ("p (a b) -> p a b", dict(a=KC)))
        MHb = ubf(4096, 2048, ("p (a b) -> p a b", dict(a=KC)))
        KM = [ubf(6144 + hh * 256, 256) for hh in range(MH)]
        VM = ubf(7168, 1024, ("p (a b) -> p a b", dict(a=2)))
        P.dma("sp", MT, memT.rearrange("(a p) m -> p a m", p=128))
        R = rstd_from([MT[:, i, :] for i in range(KC)], ONES, D)
        for i in range(KC):
            P.stt(MHb[:, i, :], MT[:, i, :], pcol(l, C_GMEM + i), R, ALU.mult, ALU.mult)
        for hh in range(MH):
            w = wload(("memkv", l, hh))
            pk = rot()
            proj_fm(w, [MHb[:, kc, :] for kc in range(KC)], pk[:, 0:256])
            R = rstd_from([pk[:, 0:256]], ONES, 128)
            P.stt(KM[hh], pk[:, 0:256], pcol(l, C_MKG), R, ALU.mult, ALU.mult)
        for hh in range(MH):
            w = wload(("memkv", l, 4 + hh))
            pv = rot()
            for mb in range(2):
                for kc in range(KC):
                    P.mm(pv[:, mb * 128:(mb + 1) * 128], MHb[:, kc, mb * 128:(mb + 1) * 128],
                         w[:, kc * 128:(kc + 1) * 128], start=(kc == 0), stop=(kc == KC - 1))
            P.act(VM[:, :, hh * 128:(hh + 1) * 128], V(pv.ap[:, 0:256].rearrange("p (a b) -> p a b", a=2), pv.bufs), AF.Copy)

        OPs = [ubf(8192, 2048, ("p (a b) -> p a b", dict(a=4))), ubf(1536, 2048, ("p (a b) -> p a b", dict(a=4)))]
        OMs = [ubf(10240, 2048, ("p (a b) -> p a b", dict(a=4))), ubf(3584, 2048, ("p (a b) -> p a b", dict(a=4)))]
        MG = ubf(12288, 4096, ("p (a b) -> p a b", dict(a=8)))
        PA = uf32(16384, 1088)
        PB = uf32(17472, 1088)
        CAR = V(CARt[:], [Buf()])
        PD = ubf(18688, 512)
        GT = [ubf(k * 512, 512) for k in range(3)]
        MQ = ubf(16384, 512)
        PMm = ubf(16896, 1024, ("p (a b) -> p a b", dict(a=2)))
        WPW = V(WPWt[:], [Buf()])
        widx = len(unit_keys); unit_keys.append(("poolw", l))
        wpo = WPW.ap
        P.op("pool", (lambda idx_: (lambda e: e.dma_start(out=wpo, in_=holder["w"][idx_][:, 0:512])))(widx), [], [WPW], dma=True)
        r2 = {"n": 0}

        rotbanks["b"] = (1, 3, 4, 5, 6, 7)
        rot2 = rot

        def streamA(tb):
            OPb = OPs[tb % 2]; OMb = OMs[tb % 2]
            for g in range(4):
                wgt = 2 ** (g + 1)
                wp = wload(("win", l, 24 + g))
                pp = rot2()
                proj_fm(wp, [H[kc][tb] for kc in range(KC)], pp)
                if tb == 0:
                    P.memset(PA[:, 0:16], 0.0)
                else:
                    P.act(PA[:, 0:16], CAR[:, g, :], AF.Copy)
                P.copy(PA[:, 16:528], pp)
                P.act(CAR[:, g, :], PA[:, 512:528], AF.Copy)
                a, b = PA, PB
                s_ = 1
                while s_ < wgt:
                    P.tt(b[:, s_:528], a[:, s_:528], a[:, 0:528 - s_], ALU.add)
                    P.act(b[:, 0:s_], a[:, 0:s_], AF.Copy)
                    a, b = b, a
                    s_ *= 2
                P.stt(PD, a[:, 16:528], 1.0 / wgt, pp, ALU.mult, ALU.subtract)
                if tb == 0:
                    n1 = wgt - 1
                    t32 = b[:, 0:n1]
                    P.tt(t32, a[:, 16:16 + n1], CT[:, K_INVC:K_INVC + n1], ALU.mult)
                    P.tt(PD[:, 0:n1], t32, pp[:, 0:n1], ALU.subtract)
                py = rot2()
                P.mm(py, WPW[:, g * 128:(g + 1) * 128], PD)
                P.ts(OPb[:, g, :], py, pcol(l, C_PSC + g), None, ALU.mult)
                yield
            for hh in range(MH):
                wm = wload(("win", l, 28 + hh))
                pq = rot2()
                proj_fm(wm, [H[kc][tb] for kc in range(KC)], pq)
                R = rstd_from([pq], ONES, 128)
                P.stt(MQ, pq, pcol(l, C_MQG), R, ALU.mult, ALU.mult)
                for mb in range(2):
                    sc = rot2()
                    P.mm(sc, KM[hh][:, mb * 128:(mb + 1) * 128], MQ)
                    P.act(PMm[:, mb, :], sc, AF.Exp, scale=128 ** -0.5)
                acc = PS[0]; sm = PS[2]
                for mb in range(2):
                    P.mm(acc, VM[:, mb, hh * 128:(hh + 1) * 128], PMm[:, mb, :], start=(mb == 0), stop=(mb == 1))
                for mb in range(2):
                    P.mm(sm, ONES, PMm[:, mb, :], start=(mb == 0), stop=(mb == 1))
                k = cnt["lt"] % 2; cnt["lt"] += 1
                P.recip(RT[k], sm)
                P.tt(OMb[:, hh, :], acc, RT[k], ALU.mult)
                yield

        def streamB(tb):
            OPb = OPs[tb % 2]; OMb = OMs[tb % 2]
            for i in range(KC):
                for b_ in range(3):
                    wg = wload(("gate", l, b_ * 8 + i))
                    pg = rot2()
                    proj_fm(wg, [H[kc][tb] for kc in range(KC)], pg)
                    P.act(GT[b_], pg, AF.Sigmoid, bias=pcol(l, C_BG + b_ * 8 + i))
                    yield
                wa = wload(("brda", l, i))
                pa = rot2()
                proj_fm(wa, [O[kc][tb] for kc in range(KC)], pa)
                P.tt(LT[0], pa, GT[0], ALU.mult)
                yield
                wb_ = wload(("brpm", l, i))
                pb = rot2()
                proj_fm(wb_, [OPb[:, kc, :] for kc in range(4)], pb)
                P.tt(LT[1], pb, GT[1], ALU.mult)
                P.tt(LT[0], LT[0], LT[1], ALU.add)
                pc = rot2()
                for kc in range(4):
                    P.mm(pc, wb_[:, (4 + kc) * 128:(5 + kc) * 128], OMb[:, kc, :], start=(kc == 0), stop=(kc == 3))
                P.tt(LT[1], pc, GT[2], ALU.mult)
                P.tt(MG[:, i, :], LT[0], LT[1], ALU.add)
                yield
            for i in range(KC):
                wo = wload(("wout", l, i))
                po = rot2()
                proj_fm(wo, [MG[:, kc, :] for kc in range(KC)], po)
                P.tt(X[i][tb], po, X[i][tb], ALU.add)
                yield

        for _ in streamA(0): pass
        for tb in range(TB):
            ga = streamA(tb + 1) if tb + 1 < TB else iter(())
            nb = 0
            for _ in streamB(tb):
                nb += 1
                if nb % 6 == 0:
                    next(ga, None)
            for _ in ga: pass
        rotbanks["b"] = (4, 5, 6, 7)
        if stop_after in ("mixer", "B1", "B2", "B3", "B4", "B5") and l == n_layers - 1:
            break

        rmsnorm_to_H(l, C_GFFN)
        ACTB = [[V(Ot[:, s_ * 4 + j, :], [O[s_ * 4 + j][t].bufs[0] for t in range(TB)]) for j in range(4)]
                for s_ in range(2)]
        SL = ubf(0, 1024, ("p (a b) -> p a b", dict(a=2)))
        if l % 2 == 0:
            experts = [None]; nff = DFF // 128
        else:
            experts = list(range(NE)); nff = DFE // 128
            RW = uf32(1024, 128)
            LG = uf32(1152, 256, ("p (a b) -> p a b", dict(a=16)))
            M8 = uf32(1408, 256, ("p (a b) -> p a b", dict(a=16)))
            CB = uf32(1664, 256, ("p (a b) -> p a b", dict(a=16)))
            E1 = uf32(1920, 256, ("p (a b) -> p a b", dict(a=16)))
            G1 = uf32(2176, 64)
            HF = uf32(2560, 1024)
            COMB = [uf32(4096 + s_ * 4096, 4096) for s_ in range(2)]
            P.dma("sp", RW, router)
            for tb in range(TB):
                R = rstd_from([X[i][tb] for i in range(KC)], ONES, D)
                pls = [rot() for _ in range(4)]
                for i in range(KC):
                    P.stt(HF, X[i][tb], pcol(l, C_GFFN + i), R, ALU.mult, ALU.mult)
                    for j in range(4):
                        P.mm(pls[j][:, 0:8], HF[:, j * 128:(j + 1) * 128], RW[:, i * 8:(i + 1) * 8],
                             start=(i == 0), stop=(i == KC - 1))
                for j in range(4):
                    P.copy(LG[:, tb * 4 + j, :], pls[j][:, 0:8])
            bc = lambda v: V(v.ap.unsqueeze(2).to_broadcast([128, 16, 8]), v.bufs)
            M1 = G1[:, 0:16]; M2 = G1[:, 16:32]
            P.reduce_max(M1, LG)
            P.tt(E1, LG, bc(M1), ALU.is_equal)
            P.stt(M8, E1, -1e30, LG, ALU.mult, ALU.add)
            P.reduce_max(M2, M8)
            P.tt(M8, M8, bc(M2), ALU.is_equal)
            GG = uf32(2240, 64)
            P.tt(GG[:, 0:16], M2, M1, ALU.subtract)
            P.act(GG[:, 16:32], GG[:, 0:16], AF.Exp)
            P.act(GG[:, 0:16], GG[:, 16:32], AF.Identity, bias=1.0)
            P.recip(GG[:, 16:32], GG[:, 0:16])
            P.act(GG[:, 0:16], GG[:, 16:32], AF.Identity, bias=1.0, scale=-1.0)
            P.tt(CB, E1, bc(GG[:, 16:32]), ALU.mult)
            P.tt(M8, M8, bc(GG[:, 0:16]), ALU.mult)
            P.tt(CB, CB, M8, ALU.add)
        ngroups = (nff + 3) // 4
        gi = 0
        for e in experts:
            if e is not None:
                cbv = COMB[e % 2]
                for tt_ in range(16):
                    if tt_ % 4 == 0: pc = rot()
                    P.mm(pc[:, (tt_ % 4) * 128:(tt_ % 4 + 1) * 128],
                         V(CB.ap[:, tt_, e:e + 1].to_broadcast([128, 128]), CB.bufs),
                         CT[:, K_ID:K_ID + 128])
                    if tt_ % 4 == 3:
                        P.act(cbv[:, (tt_ // 4) * 512:(tt_ // 4 + 1) * 512], pc, AF.Copy)
            for g in range(ngroups):
                js = list(range(g * 4, min(nff, g * 4 + 4)))
                ab = ACTB[gi % 2]; gi += 1
                for jj, j in enumerate(js):
                    w1 = wload(("w1", l, e, j))
                    w3 = wload(("w3", l, e, j))
                    for tb in range(TB):
                        p1 = rot(); p3 = rot()
                        proj_fm(w1, [H[kc][tb] for kc in range(KC)], p1)
                        proj_fm(w3, [H[kc][tb] for kc in range(KC)], p3)
                        sl = SL[:, tb % 2, :]
                        P.act(sl, p1, AF.Silu)
                        dst = ab[jj][:, tb * 512:(tb + 1) * 512]
                        if e is None:
                            P.tt(dst, p3, sl, ALU.mult)
                        else:
                            k = cnt["lt"] % 2; cnt["lt"] += 1
                            P.tt(LT[k], p3, sl, ALU.mult)
                            P.tt(dst, LT[k], cbv[:, tb * 512:(tb + 1) * 512], ALU.mult)
                w2s = [wload(("w2", l, e, j)) for j in js]
                for i in range(KC):
                    for tb in range(TB):
                        py = PS[(i * TB + tb) % 4]
                        for jj in range(len(js)):
                            P.mm(py, w2s[jj][:, i * 128:(i + 1) * 128], ab[jj][:, tb * 512:(tb + 1) * 512],
                                 start=(jj == 0), stop=(jj == len(js) - 1))
                        P.tt(X[i][tb], py, X[i][tb], ALU.add)

    if dump is not None:
        dump(P, dict(X=X, H=H, O=O), dump_out)
    for i in range(KC):
        for tb in range(TB):
            final_ops.append(P.dma("sp", outT[i * 128:(i + 1) * 128, tb * 512:(tb + 1) * 512], X[i][tb]))
    holder["w"] = nc.dram_tensor("wbig", [max(1, len(unit_keys)), 128, 1024], F32, kind="ExternalInput").ap()
    P.emit(final_ops)
    es.close()
    return nc, unit_keys


def _blk_cols(W, j):
    K = W.shape[0]
    kc = K // 128
    return W[:, j * 128:(j + 1) * 128].reshape(kc, 128, 128).transpose(1, 0, 2).reshape(128, kc * 128)


def get_unit(key, inp):
    kind = key[0]; l = key[1]
    if kind == "win": return _blk_cols(inp["w_in"][l], key[2])
    if kind == "gate": return _blk_cols(inp["w_gate"][l], key[2])
    if kind == "memkv": return _blk_cols(inp["w_mem_kv"][l], key[2])
    if kind == "brda": return _blk_cols(inp["w_br_da"][l], key[2])
    if kind == "wout": return _blk_cols(inp["w_out"][l], key[2])
    if kind == "brpm":
        return np.concatenate([_blk_cols(inp["w_br_pool"][l], key[2]), _blk_cols(inp["w_br_mem"][l], key[2])], axis=1)
    if kind == "poolw":
        u = np.zeros((128, 1024), np.float32)
        u[:, 0:512] = inp["pool_w"][l].transpose(1, 0, 2).reshape(128, 512)
        return u
    e, j = key[2], key[3]
    if kind in ("w1", "w3"):
        if e is None: W = inp["ffn_" + kind][l // 2]
        else: W = inp["moe_" + kind][l // 2][e]
        return _blk_cols(W, j)
    if kind == "w2":
        if e is None: W = inp["ffn_w2"][l // 2]
        else: W = inp["moe_w2"][l // 2][e]
        return W[j * 128:(j + 1) * 128, :]
    raise KeyError(key)


def make_params(inp):
    PRM = np.zeros((128, DEPTH * NPC), np.float32)
    fm = lambda v: np.asarray(v, np.float32).reshape(-1, 128).T
    for l in range(DEPTH):
        b = l * NPC
        PRM[:, b + 0:b + 8] = fm(inp["norm_mix"][l])
        PRM[:, b + 8:b + 16] = fm(inp["norm_mem"][l])
        PRM[:, b + 16:b + 24] = fm(inp["norm_ffn"][l])
        PRM[:, b + 24:b + 48] = fm(inp["b_gate"][l])
        PRM[:, b + 48] = np.asarray(inp["da_q_norm"][l]).reshape(128)
        PRM[:, b + 49] = np.asarray(inp["da_k_norm"][l]).reshape(128)
        PRM[:, b + 50] = np.asarray(inp["da_out_norm"][l]).reshape(128)
        PRM[:, b + 51:b + 55] = fm(inp["pool_scale"][l])
        PRM[:, b + 55] = np.asarray(inp["mem_q_norm"][l]).reshape(128)
        PRM[:, b + 56] = np.asarray(inp["mem_k_norm"][l]).reshape(128)
        for j, nm in enumerate(["da_lam_q1", "da_lam_k1", "da_lam_q2", "da_lam_k2"]):
            PRM[:, b + 57 + 64 * j:b + 57 + 64 * (j + 1)] = np.broadcast_to(np.asarray(inp[nm][l]).reshape(1, 64), (128, 64))
    return PRM


def make_consts():
    C = np.zeros((128, NCC), np.float32)
    sl = alibi_slopes()
    kp = np.arange(128, dtype=np.float64)
    for h in range(NH):
        for dd in range(-3, 16):
            C[:, h * 20 + dd + 3] = sl[h] * (kp - 128.0 * dd - GROUPW[h] / 2.0)
        qp = np.arange(128, dtype=np.float64)[None, :]
        kk = kp[:, None]
        allowed = (kk // 64) <= (qp // 64)
        B = -sl[h] * np.abs(qp - kk) + sl[h] * (qp - 64.0)
        C[:, 160 + h * 128:160 + (h + 1) * 128] = np.where(allowed, B, -30000.0)
    C[:, 160 + 1024:160 + 1024 + 16] = (1.0 / (np.arange(16) + 1.0))[None, :]
    C[:, 160 + 1024 + 16:] = np.eye(128)
    return C


_CACHE = {}


def _get_program():
    if "p" not in _CACHE:
        _CACHE["p"] = build_program()
    return _CACHE["p"]


def pack_weights(unit_keys, inp):
    wb = np.empty((len(unit_keys), 128, 1024), np.float32)
    for n, k in enumerate(unit_keys):
        wb[n] = get_unit(k, inp)
    return wb


def kernel(**inputs):
    inp = {k: np.asarray(v) for k, v in inputs.items()}
    nc, unit_keys = _get_program()
    wb = pack_weights(unit_keys, inp)
    PRM = make_params(inp)
    CST = make_consts()
    rt = np.ascontiguousarray(inp["moe_router"][0].reshape(KC, 128, NE).transpose(1, 0, 2).reshape(128, KC * NE))
    in_maps = []
    for b in range(8):
        in_maps.append({
            "xT": np.ascontiguousarray(inp["x"][b].T),
            "memT": np.ascontiguousarray(inp["mem"][b].T),
            "params": PRM, "consts": CST, "router": rt, "wbig": wb,
        })
    res = run_bass_kernel_spmd(nc, in_maps, core_ids=list(range(8)))
    out = np.stack([np.asarray(r["outT"]).T for r in res.results], axis=0)
    return out.astype(np.float32)
```

```python
import math
import numpy as np
import concourse.bass as bass
import concourse.mybir as mybir
from concourse.bass_utils import run_bass_kernel_spmd

F32 = mybir.dt.float32
BF16 = mybir.dt.bfloat16
AF = mybir.ActivationFunctionType
ALU = mybir.AluOpType

D = 1024; S = 2048; KC = 8; TB = 4; DEPTH = 2
NH = 8; MEM = 256; MH = 4
DFF = 2816; NE = 8; DFE = 3584
EPS = 1e-6
NSLOT = 8
NPC = 320
NCC = 8 * 20 + 8 * 128 + 16 + 128


GROUPW = (128, 256, 512, 512, 512, 512, 512, 512)


def alibi_slopes():
    return [2.0 ** (-8.0 * (h + 1) / NH) for h in range(NH)]


def lambda_init(layer):
    return 0.8 - 0.6 * math.exp(-0.3 * layer)


class Buf:
    __slots__ = ("w", "r", "rd")

    def __init__(self):
        self.w = None; self.r = {}; self.rd = []


class V:
    __slots__ = ("ap", "bufs")

    def __init__(self, ap, bufs):
        self.ap = ap; self.bufs = bufs

    def __getitem__(self, idx):
        return V(self.ap[idx], self.bufs)


class Op:
    __slots__ = ("eng", "fn", "deps", "sig", "sem", "val", "dma", "prev", "wr")


COMPUTE = ("pe", "act", "dve", "pool")
_DSZ = {F32: 4, BF16: 2}


def _byte_range(ap):
    try:
        sz = _DSZ[ap.dtype]
        dims = list(ap.ap)[1:]
        span = 1
        for st, cnt in dims:
            if st < 0: return None
            span += (cnt - 1) * st
        pstride = list(ap.ap)[0][0]
        off = ap.offset % pstride if pstride else ap.offset
        return (ap.tensor.name, off * sz, (off + span) * sz)
    except Exception:
        return None


class Prog:
    def __init__(self, nc):
        self.nc = nc
        self.ops = {e: [] for e in ("pe", "act", "dve", "pool", "sp")}

    def op(self, eng, fn, reads=(), writes=(), dma=False):
        o = Op(); o.eng = eng; o.fn = fn; o.dma = dma; o.sig = dma; o.sem = None; o.val = 0; o.prev = None
        o.wr = _byte_range(writes[0].ap) if (len(writes) == 1 and isinstance(writes[0], V)) else None
        deps = []
        rb = []; wb = []
        for v in reads: rb.extend(v.bufs)
        for v in writes: wb.extend(v.bufs)
        for b in rb:
            if b.w is not None: deps.append((b.w, 0))
        for b in wb:
            if b.w is not None: deps.append((b.w, 1))
            for r in b.r.values(): deps.append((r, 2))
            for r in b.rd: deps.append((r, 2))
        keep = {}
        for d, kind in deps:
            if d is o: continue
            if d.eng == eng and not dma and not d.dma:
                if eng == "pe": continue
                if kind == 1 and o.wr is not None and d.wr is not None and \
                        (o.wr[0] != d.wr[0] or o.wr[1] >= d.wr[2] or d.wr[1] >= o.wr[2]):
                    continue
            keep[id(d)] = d
        o.deps = list(keep.values())
        for d in o.deps: d.sig = True
        for b in rb:
            if dma: b.rd.append(o)
            else: b.r[eng] = o
        for b in wb:
            b.w = o; b.r = {}; b.rd = []
        self.ops[eng].append(o)
        return o

    def mm(self, out, lhsT, rhs, start=True, stop=True):
        o, l, r = out.ap, lhsT.ap, rhs.ap
        return self.op("pe", lambda e: e.matmul(o, lhsT=l, rhs=r, start=start, stop=stop),
                       [lhsT, rhs], [out])

    def act(self, out, in_, func, bias=None, scale=1.0, eng="act"):
        o, i = out.ap, in_.ap
        rd = [in_]
        if isinstance(bias, V):
            rd.append(bias); b = bias.ap
        else:
            b = 0.0 if bias is None else float(bias)
        if isinstance(scale, V):
            rd.append(scale); s = scale.ap
        else:
            s = float(scale)
        return self.op(eng, lambda e: e.activation(out=o, in_=i, func=func, bias=b, scale=s), rd, [out])

    def stt(self, out, in0, scalar, in1, op0, op1, eng="dve"):
        o, a, c = out.ap, in0.ap, in1.ap
        rd = [in0, in1]
        if isinstance(scalar, V):
            rd.append(scalar); s = scalar.ap
        else:
            s = float(scalar)
        return self.op(eng, lambda e: e.scalar_tensor_tensor(out=o, in0=a, scalar=s, in1=c, op0=op0, op1=op1),
                       rd, [out])

    def tt(self, out, in0, in1, op, eng="dve"):
        o, a, c = out.ap, in0.ap, in1.ap
        return self.op(eng, lambda e: e.tensor_tensor(out=o, in0=a, in1=c, op=op), [in0, in1], [out])

    def ts(self, out, in0, s1, s2, op0, op1=None, eng="dve"):
        o, a = out.ap, in0.ap
        rd = [in0]
        if isinstance(s1, V): rd.append(s1); s1 = s1.ap
        if isinstance(s2, V): rd.append(s2); s2 = s2.ap
        if op1 is None:
            return self.op(eng, lambda e: e.tensor_scalar(out=o, in0=a, scalar1=s1, scalar2=None, op0=op0), rd, [out])
        return self.op(eng, lambda e: e.tensor_scalar(out=o, in0=a, scalar1=s1, scalar2=s2, op0=op0, op1=op1), rd, [out])

    def copy(self, out, in_, eng="dve"):
        o, i = out.ap, in_.ap
        return self.op(eng, lambda e: e.tensor_copy(out=o, in_=i), [in_], [out])

    def recip(self, out, in_):
        o, i = out.ap, in_.ap
        return self.op("dve", lambda e: e.reciprocal(out=o, in_=i), [in_], [out])

    def memset(self, out, val, eng="dve"):
        o = out.ap
        return self.op(eng, lambda e: e.memset(o, val), [], [out])

    def reduce_sum(self, out, in_):
        o, i = out.ap, in_.ap
        return self.op("dve", lambda e: e.reduce_sum(out=o, in_=i, axis=mybir.AxisListType.X), [in_], [out])

    def reduce_max(self, out, in_):
        o, i = out.ap, in_.ap
        return self.op("dve", lambda e: e.reduce_max(out=o, in_=i, axis=mybir.AxisListType.X), [in_], [out])

    def dma(self, q, out, in_, reads=(), writes=()):
        o = out.ap if isinstance(out, V) else out
        i = in_.ap if isinstance(in_, V) else in_
        rd = list(reads); wr = list(writes)
        if isinstance(in_, V): rd.append(in_)
        if isinstance(out, V): wr.append(out)
        return self.op(q, lambda e: e.dma_start(out=o, in_=i), rd, wr, dma=True)

    def emit(self, final_ops):
        nc = self.nc
        from contextlib import ExitStack
        NSD = {"sp": 12, "pool": 12, "act": 2, "dve": 1, "pe": 1}
        with ExitStack() as es:
            esem = {e: es.enter_context(nc.semaphore("s_" + e)) for e in COMPUTE}
            dsem = {q: [es.enter_context(nc.semaphore("d_%s%d" % (q, k))) for k in range(NSD[q])]
                    for q in ("sp", "pool")}
            for e, lst in self.ops.items():
                c = 0; nd = 0; dlist = []
                for o in lst:
                    if o.dma:
                        ns = len(dsem[e])
                        o.sem = dsem[e][nd % ns]; o.val = 16 * (nd // ns + 1)
                        if nd >= ns: o.prev = dlist[nd - ns]
                        dlist.append(o); nd += 1
                    elif o.sig:
                        c += 1; o.sem = esem[e]; o.val = c
            block = es.enter_context(nc.Block())
            prog = self

            def run(engname, e):
                waited = {}
                lst = prog.ops[engname]
                for o in lst:
                    need = {}
                    deps = o.deps if o.prev is None else o.deps + [o.prev]
                    for d in deps:
                        k = id(d.sem)
                        if waited.get(k, 0) >= d.val: continue
                        if k not in need or need[k][1] < d.val: need[k] = (d.sem, d.val)
                    for k, (sem, val) in need.items():
                        e.wait_ge(sem, val); waited[k] = val
                    inst = o.fn(e)
                    if o.sig:
                        inst.then_inc(o.sem, 16 if o.dma else 1)
                if engname == "sp":
                    for o in final_ops:
                        if waited.get(id(o.sem), 0) < o.val:
                            e.wait_ge(o.sem, o.val); waited[id(o.sem)] = o.val

            @block.tensor
            def _(e): run("pe", e)

            @block.scalar
            def _(e): run("act", e)

            @block.vector
            def _(e): run("dve", e)

            @block.gpsimd
            def _(e): run("pool", e)

            @block.sync
            def _(e): run("sp", e)


def build_program(n_layers=DEPTH, stop_after=None, dump=None):
    nc = bass.Bass("TRN2", target_bir_lowering=False)
    P = Prog(nc)
    unit_keys = []
    from contextlib import ExitStack
    es = ExitStack()

    xT = nc.dram_tensor("xT", [D, S], F32, kind="ExternalInput").ap()
    memT = nc.dram_tensor("memT", [D, MEM], F32, kind="ExternalInput").ap()
    params = nc.dram_tensor("params", [128, DEPTH * NPC], F32, kind="ExternalInput").ap()
    consts = nc.dram_tensor("consts", [128, NCC], F32, kind="ExternalInput").ap()
    router = nc.dram_tensor("router", [128, KC * NE], F32, kind="ExternalInput").ap()
    holder = {}
    outT = nc.dram_tensor("outT", [D, S], F32, kind="ExternalOutput").ap()

    def sb(name, shape, dt):
        return es.enter_context(nc.sbuf_tensor(name, shape, dt))

    def ps(name):
        return es.enter_context(nc.psum_tensor(name, [128, 512], F32))

    Xt = sb("X", [128, KC, S], F32)
    Ht = sb("H", [128, KC, S], BF16)
    Ot = sb("O", [128, KC, S], BF16)
    UN = 38 * 512
    Ut = sb("U", [128, UN], BF16)
    Wt = sb("W", [128, NSLOT, 1024], BF16)
    SQt = sb("SQ", [128, 4, 512], BF16)
    LTt = sb("LT", [128, 2, 512], F32)
    RTt = sb("RT", [128, 2, 512], F32)
    PRt = sb("PR", [128, DEPTH * NPC], F32)
    CTt = sb("CT", [128, NCC], F32)
    ONEt = sb("ONES", [128, 128], BF16)
    BDt = sb("BD64", [128, 128], BF16)
    SMt = sb("SM", [128, 64], F32)
    CARt = sb("CAR", [128, 4, 16], F32)
    WPWt = sb("WPW", [128, 512], BF16)
    PSt = [ps("ps%d" % k) for k in range(8)]

    def grid(t, n1, n2, w):
        return [[V(t[:, a, b * w:(b + 1) * w], [Buf()]) for b in range(n2)] for a in range(n1)]

    X = grid(Xt, KC, TB, 512)
    H = grid(Ht, KC, TB, 512)
    O = grid(Ot, KC, TB, 512)
    PS = [V(p[:], [Buf()]) for p in PSt]
    Wslot = [V(Wt[:, k, :], [Buf()]) for k in range(NSLOT)]
    SQ = [V(SQt[:, k, :], [Buf()]) for k in range(4)]
    LT = [V(LTt[:, k, :], [Buf()]) for k in range(2)]
    RT = [V(RTt[:, k, :], [Buf()]) for k in range(2)]
    PR = V(PRt[:], [Buf()])
    CT = V(CTt[:], [Buf()])
    ONES = V(ONEt[:], [Buf()])
    BD64 = V(BDt[:], [Buf()])
    SM = V(SMt[:], [Buf()])
    UG = [Buf() for _ in range(UN // 512)]

    def ubf(off, n, shape=None):
        ap = Ut[:, off:off + n]
        if shape: ap = ap.rearrange(shape[0], **shape[1])
        return V(ap, UG[off // 512:(off + n + 511) // 512])

    def uf32(off, n, shape=None):
        ap = Ut[:, off:off + n].bitcast(F32)
        if shape: ap = ap.rearrange(shape[0], **shape[1])
        return V(ap, UG[off // 512:(off + n + 511) // 512])

    cnt = {"rot": 0, "sq": 0, "lt": 0, "w": 0}

    rotbanks = {"b": (4, 5, 6, 7)}

    def rot():
        bb = rotbanks["b"]
        k = bb[cnt["rot"] % len(bb)]; cnt["rot"] += 1
        return PS[k]

    def wload(key):
        idx = len(unit_keys); unit_keys.append(key)
        slot = Wslot[cnt["w"] % NSLOT]; cnt["w"] += 1
        so = slot.ap
        P.op("pool", lambda e: e.dma_start(out=so, in_=holder["w"][idx]), [], [slot], dma=True)
        return slot

    def pcol(l, c, n=1):
        return PR[:, l * NPC + c: l * NPC + c + n]

    C_GMIX, C_GMEM, C_GFFN, C_BG, C_QG, C_KG, C_OG, C_PSC, C_MQG, C_MKG, C_LAM = 0, 8, 16, 24, 48, 49, 50, 51, 55, 56, 57
    K_BOFF, K_BDIAG, K_INVC, K_ID = 0, 160, 160 + 1024, 160 + 1024 + 16

    P.dma("sp", PR, params)
    P.dma("sp", CT, consts)
    for i in range(KC):
        for tb in range(TB):
            P.dma("sp", X[i][tb], xT[i * 128:(i + 1) * 128, tb * 512:(tb + 1) * 512])
    P.memset(ONES, 1.0)
    P.memset(BD64, 0.0)
    P.memset(BD64[0:64, 0:64], 1.0)
    P.memset(BD64[64:128, 64:128], 1.0)

    def rstd_from(srcs, ones, n, lnbias=0.0, sbuf_src=0):
        N = srcs[0].ap.shape[-1]
        ss = rot()
        for i, s_ in enumerate(srcs):
            sq = SQ[cnt["sq"] % 4]; cnt["sq"] += 1
            if sbuf_src == 2 or (sbuf_src == 1 and i % 2 == 1):
                P.tt(sq[:, 0:N], s_, s_, ALU.mult)
            else:
                P.act(sq[:, 0:N], s_, AF.Square)
            P.mm(ss[:, 0:N], ones, sq[:, 0:N], start=(i == 0), stop=(i == len(srcs) - 1))
        k = cnt["lt"] % 2; cnt["lt"] += 1
        P.act(RT[k][:, 0:N], ss[:, 0:N], AF.Ln, bias=EPS, scale=1.0 / n)
        P.act(RT[k][:, 0:N], RT[k][:, 0:N], AF.Exp, bias=lnbias, scale=-0.5)
        return RT[k][:, 0:N]

    def rmsnorm_to_H(l, gcol):
        for tb in range(TB):
            R = rstd_from([X[i][tb] for i in range(KC)], ONES, D, sbuf_src=1)
            for i in range(KC):
                P.stt(H[i][tb], X[i][tb], pcol(l, gcol + i), R, ALU.mult, ALU.mult)

    def proj_fm(w, rhs_list, out_ps, kcs=None):
        n = len(rhs_list)
        for kc in range(n):
            P.mm(out_ps, w[:, kc * 128:(kc + 1) * 128], rhs_list[kc], start=(kc == 0), stop=(kc == n - 1))

    final_ops = []

    def dump_out(name, views):
        for j, v in enumerate(views):
            n = v.ap.shape[-1]
            t = nc.dram_tensor("%s_%d" % (name, j), [128, n], F32, kind="ExternalOutput").ap()
            final_ops.append(P.dma("pool", t, v))

    slopes = alibi_slopes()

    for l in range(n_layers):
        lam0 = lambda_init(l)
        lv = [pcol(l, C_LAM + 64 * j, 64) for j in range(4)]
        tmp = uf32(0, 128)
        P.tt(tmp, lv[0], lv[1], ALU.mult)
        P.reduce_sum(SM[:, 1:2], tmp)
        P.tt(tmp, lv[2], lv[3], ALU.mult)
        P.reduce_sum(SM[:, 2:3], tmp)
        P.act(SM[:, 3:5], SM[:, 1:3], AF.Exp)
        P.tt(SM[:, 5:6], SM[:, 4:5], SM[:, 3:4], ALU.subtract)
        P.act(SM[:, 0:1], SM[:, 5:6], AF.Identity, bias=-lam0)

        rmsnorm_to_H(l, C_GMIX)
        if stop_after == "norm1" and l == n_layers - 1:
            break

        QN = [[ubf((s_ * 4 + tb) * 512, 512) for tb in range(TB)] for s_ in range(2)]
        KN = [[ubf(4096 + (s_ * 4 + tb) * 512, 512) for tb in range(TB)] for s_ in range(2)]
        VH = [[ubf(8192 + (s_ * 4 + tb) * 512, 512, ("p (a b) -> p a b", dict(a=4))) for tb in range(TB)]
              for s_ in range(2)]
        PT = [ubf(12288 + s_ * 1024, 1024, ("p (a b) -> p a b", dict(a=2))) for s_ in range(2)]
        TMP = [uf32(14336 + s_ * 512, 256) for s_ in range(2)]
        FR = [uf32(15360 + k * 1024, 1024) for k in range(4)]
        def proj_chunks(h):
            st = h % 2
            box = {}
            chunks = []

            def qk_chunk(which, tb):
                def f():
                    if tb == 0:
                        box[which] = wload(("win", l, (0 if which == "q" else 8) + h))
                    pq = rot()
                    proj_fm(box[which], [H[kc][tb] for kc in range(KC)], pq)
                    R = rstd_from([pq], BD64, 64)
                    dst = QN[st][tb] if which == "q" else KN[st][tb]
                    P.stt(dst, pq, pcol(l, C_QG if which == "q" else C_KG), R, ALU.mult, ALU.mult)
                return f

            def v_chunk(tb):
                def f():
                    if tb == 0:
                        box["v"] = wload(("win", l, 16 + h))
                    wv = box["v"]
                    pv = rot()
                    for j in range(4):
                        for kc in range(KC):
                            P.mm(pv[:, j * 128:(j + 1) * 128], H[kc][tb][:, j * 128:(j + 1) * 128],
                                 wv[:, kc * 128:(kc + 1) * 128], start=(kc == 0), stop=(kc == KC - 1))
                    P.copy(VH[st][tb], V(pv.ap.rearrange("p (a b) -> p a b", a=4), pv.bufs))
                return f
            for tb in range(TB): chunks.append(qk_chunk("q", tb))
            for tb in range(TB): chunks.append(qk_chunk("k", tb))
            for tb in range(TB): chunks.append(v_chunk(tb))
            return chunks

        for c in proj_chunks(0): c()
        for h in range(NH):
            st = h % 2
            nxt = proj_chunks(h + 1) if h + 1 < NH else []
            iters = [(G, kb) for G in range(TB) for kb in range(4 * G + 4)]
            ACC = [PS[0], PS[1]]; SUM = [PS[2], PS[3]]

            def emit_S(G, kb, par):
                j0 = max(0, kb - 4 * G); c0 = j0 * 128
                Sm = [PS[4 + 2 * par], PS[5 + 2 * par]]
                kt = KN[st][kb // 4]; kc0 = (kb % 4) * 128
                for m in range(2):
                    P.mm(Sm[m][:, c0:512], kt[64 * m:64 * m + 64, kc0:kc0 + 128],
                         QN[st][G][64 * m:64 * m + 64, c0:512])
                return Sm
            rotbanks["b"] = (6, 7)
            S_next = emit_S(*iters[0], 0)
            for n, (G, kb) in enumerate(iters):
                Sm = S_next
                if n + 1 < len(iters): S_next = emit_S(*iters[n + 1], (n + 1) % 2)
                rotbanks["b"] = (4 + 2 * (n % 2), 5 + 2 * (n % 2))
                nkb = 4 * G + 4
                j0 = max(0, kb - 4 * G); c0 = j0 * 128
                Pm = PT[n % 2]
                Wh = GROUPW[h]; nbk = Wh // 128
                for m in range(2):
                    for u in range(512 // Wh):
                        jb0 = u * nbk; jb1 = jb0 + nbk
                        if jb1 <= j0: continue
                        if kb < 4 * G + jb0:
                            dd = 4 * G + jb0 - kb
                            cs = slice(jb0 * 128, jb1 * 128)
                            P.act(Pm[:, m, cs], Sm[m][:, cs], AF.Exp,
                                  bias=CT[:, K_BOFF + h * 20 + dd + 3:K_BOFF + h * 20 + dd + 4], scale=0.125)
                        else:
                            jk = kb - 4 * G; jl = jk - jb0
                            cs = slice(jk * 128, (jk + 1) * 128)
                            t_ = TMP[m]
                            P.stt(t_, Sm[m][:, cs], 0.125, CT[:, K_BDIAG + h * 128:K_BDIAG + (h + 1) * 128],
                                  ALU.mult, ALU.add)
                            P.act(Pm[:, m, cs], t_, AF.Exp, bias=slopes[h] * (128.0 * jl - Wh / 2.0 + 64.0))
                            if jk + 1 < jb1:
                                cs = slice((jk + 1) * 128, jb1 * 128)
                                P.act(Pm[:, m, cs], Sm[m][:, cs], AF.Exp,
                                      bias=CT[:, K_BOFF + h * 20 - jl + 3:K_BOFF + h * 20 - jl + 4], scale=0.125)
                for m in range(2):
                    P.mm(ACC[m][:, c0:512], VH[st][kb // 4][:, kb % 4, :], Pm[:, m, c0:512],
                         start=(kb == 0), stop=(kb == nkb - 1))
                    P.mm(SUM[m][:, c0:512], ONES, Pm[:, m, c0:512], start=(kb == 0), stop=(kb == nkb - 1))
                if n % 3 == 1 and nxt:
                    nxt.pop(0)()
                if kb == nkb - 1:
                    P.recip(FR[0], SUM[0]); P.recip(FR[1], SUM[1])
                    P.tt(FR[2], ACC[0], FR[0], ALU.mult)
                    P.tt(FR[3], ACC[1], FR[1], ALU.mult)
                    P.stt(FR[0], FR[3], SM[:, 0:1], FR[2], ALU.mult, ALU.add)
                    R = rstd_from([FR[0]], ONES, 128, lnbias=math.log(1.0 - lam0), sbuf_src=2)
                    P.stt(O[h][G], FR[0], pcol(l, C_OG), R, ALU.mult, ALU.mult)
            rotbanks["b"] = (4, 5, 6, 7)
            while nxt:
                nxt.pop(0)()
        if stop_after == "attn" and l == n_layers - 1:
            break

        MT = uf32(0, 4096, ("p (a b) -> p a b", dict(a=KC)))
        MHb = ubf(4096, 2048, ("p (a b) -> p a b", dict(a=KC)))
        KM = [ubf(6144 + hh * 256, 256) for hh in range(MH)]
        VM = ubf(7168, 1024, ("p (a b) -> p a b", dict(a=2)))
        P.dma("sp", MT, memT.rearrange("(a p) m -> p a m", p=128))
        R = rstd_from([MT[:, i, :] for i in range(KC)], ONES, D)
        for i in range(KC):
            P.stt(MHb[:, i, :], MT[:, i, :], pcol(l, C_GMEM + i), R, ALU.mult, ALU.mult)
        for hh in range(MH):
            w = wload(("memkv", l, hh))
            pk = rot()
            proj_fm(w, [MHb[:, kc, :] for kc in range(KC)], pk[:, 0:256])
            R = rstd_from([pk[:, 0:256]], ONES, 128)
            P.stt(KM[hh], pk[:, 0:256], pcol(l, C_MKG), R, ALU.mult, ALU.mult)
        for hh in range(MH):
            w = wload(("memkv", l, 4 + hh))
            pv = rot()
            for mb in range(2):
                for kc in range(KC):
                    P.mm(pv[:, mb * 128:(mb + 1) * 128], MHb[:, kc, mb * 128:(mb + 1) * 128],
                         w[:, kc * 128:(kc + 1) * 128], start=(kc == 0), stop=(kc == KC - 1))
            P.act(VM[:, :, hh * 128:(hh + 1) * 128], V(pv.ap[:, 0:256].rearrange("p (a b) -> p a b", a=2), pv.bufs), AF.Copy)

        OPs = [ubf(8192, 2048, ("p (a b) -> p a b", dict(a=4))), ubf(1536, 2048, ("p (a b) -> p a b", dict(a=4)))]
        OMs = [ubf(10240, 2048, ("p (a b) -> p a b", dict(a=4))), ubf(3584, 2048, ("p (a b) -> p a b", dict(a=4)))]
        MG = ubf(12288, 4096, ("p (a b) -> p a b", dict(a=8)))
        PA = uf32(16384, 1088)
        PB = uf32(17472, 1088)
        CAR = V(CARt[:], [Buf()])
        PD = ubf(18688, 512)
        GT = [ubf(k * 512, 512) for k in range(3)]
        MQ = ubf(16384, 512)
        PMm = ubf(16896, 1024, ("p (a b) -> p a b", dict(a=2)))
        WPW = V(WPWt[:], [Buf()])
        widx = len(unit_keys); unit_keys.append(("poolw", l))
        wpo = WPW.ap
        P.op("pool", (lambda idx_: (lambda e: e.dma_start(out=wpo, in_=holder["w"][idx_][:, 0:512])))(widx), [], [WPW], dma=True)
        r2 = {"n": 0}

        rotbanks["b"] = (1, 3, 4, 5, 6, 7)
        rot2 = rot

        def streamA(tb):
            OPb = OPs[tb % 2]; OMb = OMs[tb % 2]
            for g in range(4):
                wgt = 2 ** (g + 1)
                wp = wload(("win", l, 24 + g))
                pp = rot2()
                proj_fm(wp, [H[kc][tb] for kc in range(KC)], pp)
                if tb == 0:
                    P.memset(PA[:, 0:16], 0.0)
                else:
                    P.act(PA[:, 0:16], CAR[:, g, :], AF.Copy)
                P.copy(PA[:, 16:528], pp)
                P.act(CAR[:, g, :], PA[:, 512:528], AF.Copy)
                a, b = PA, PB
                s_ = 1
                while s_ < wgt:
                    P.tt(b[:, s_:528], a[:, s_:528], a[:, 0:528 - s_], ALU.add)
                    P.act(b[:, 0:s_], a[:, 0:s_], AF.Copy)
                    a, b = b, a
                    s_ *= 2
                P.stt(PD, a[:, 16:528], 1.0 / wgt, pp, ALU.mult, ALU.subtract)
                if tb == 0:
                    n1 = wgt - 1
                    t32 = b[:, 0:n1]
                    P.tt(t32, a[:, 16:16 + n1], CT[:, K_INVC:K_INVC + n1], ALU.mult)
                    P.tt(PD[:, 0:n1], t32, pp[:, 0:n1], ALU.subtract)
                py = rot2()
                P.mm(py, WPW[:, g * 128:(g + 1) * 128], PD)
                P.ts(OPb[:, g, :], py, pcol(l, C_PSC + g), None, ALU.mult)
                yield
            for hh in range(MH):
                wm = wload(("win", l, 28 + hh))
                pq = rot2()
                proj_fm(wm, [H[kc][tb] for kc in range(KC)], pq)
                R = rstd_from([pq], ONES, 128)
                P.stt(MQ, pq, pcol(l, C_MQG), R, ALU.mult, ALU.mult)
                for mb in range(2):
                    sc = rot2()
                    P.mm(sc, KM[hh][:, mb * 128:(mb + 1) * 128], MQ)
                    P.act(PMm[:, mb, :], sc, AF.Exp, scale=128 ** -0.5)
                acc = PS[0]; sm = PS[2]
                for mb in range(2):
                    P.mm(acc, VM[:, mb, hh * 128:(hh + 1) * 128], PMm[:, mb, :], start=(mb == 0), stop=(mb == 1))
                for mb in range(2):
                    P.mm(sm, ONES, PMm[:, mb, :], start=(mb == 0), stop=(mb == 1))
                k = cnt["lt"] % 2; cnt["lt"] += 1
                P.recip(RT[k], sm)
                P.tt(OMb[:, hh, :], acc, RT[k], ALU.mult)
                yield

        def streamB(tb):
            OPb = OPs[tb % 2]; OMb = OMs[tb % 2]
            for i in range(KC):
                for b_ in range(3):
                    wg = wload(("gate", l, b_ * 8 + i))
                    pg = rot2()
                    proj_fm(wg, [H[kc][tb] for kc in range(KC)], pg)
                    P.act(GT[b_], pg, AF.Sigmoid, bias=pcol(l, C_BG + b_ * 8 + i))
                    yield
                wa = wload(("brda", l, i))
                pa = rot2()
                proj_fm(wa, [O[kc][tb] for kc in range(KC)], pa)
                P.tt(LT[0], pa, GT[0], ALU.mult)
                yield
                wb_ = wload(("brpm", l, i))
                pb = rot2()
                proj_fm(wb_, [OPb[:, kc, :] for kc in range(4)], pb)
                P.tt(LT[1], pb, GT[1], ALU.mult)
                P.tt(LT[0], LT[0], LT[1], ALU.add)
                pc = rot2()
                for kc in range(4):
                    P.mm(pc, wb_[:, (4 + kc) * 128:(5 + kc) * 128], OMb[:, kc, :], start=(kc == 0), stop=(kc == 3))
                P.tt(LT[1], pc, GT[2], ALU.mult)
                P.tt(MG[:, i, :], LT[0], LT[1], ALU.add)
                yield
            for i in range(KC):
                wo = wload(("wout", l, i))
                po = rot2()
                proj_fm(wo, [MG[:, kc, :] for kc in range(KC)], po)
                P.tt(X[i][tb], po, X[i][tb], ALU.add)
                yield

        for _ in streamA(0): pass
        for tb in range(TB):
            ga = streamA(tb + 1) if tb + 1 < TB else iter(())
            nb = 0
            for _ in streamB(tb):
                nb += 1
                if nb % 6 == 0:
                    next(ga, None)
            for _ in ga: pass
        rotbanks["b"] = (4, 5, 6, 7)
        if stop_after in ("mixer", "B1", "B2", "B3", "B4", "B5") and l == n_layers - 1:
            break

        rmsnorm_to_H(l, C_GFFN)
        ACTB = [[V(Ot[:, s_ * 4 + j, :], [O[s_ * 4 + j][t].bufs[0] for t in range(TB)]) for j in range(4)]
                for s_ in range(2)]
        SL = ubf(0, 1024, ("p (a b) -> p a b", dict(a=2)))
        if l % 2 == 0:
            experts = [None]; nff = DFF // 128
        else:
            experts = list(range(NE)); nff = DFE // 128
            RW = uf32(1024, 128)
            LG = uf32(1152, 256, ("p (a b) -> p a b", dict(a=16)))
            M8 = uf32(1408, 256, ("p (a b) -> p a b", dict(a=16)))
            CB = uf32(1664, 256, ("p (a b) -> p a b", dict(a=16)))
            E1 = uf32(1920, 256, ("p (a b) -> p a b", dict(a=16)))
            G1 = uf32(2176, 64)
            HF = uf32(2560, 1024)
            COMB = [uf32(4096 + s_ * 4096, 4096) for s_ in range(2)]
            P.dma("sp", RW, router)
            for tb in range(TB):
                R = rstd_from([X[i][tb] for i in range(KC)], ONES, D)
                pls = [rot() for _ in range(4)]
                for i in range(KC):
                    P.stt(HF, X[i][tb], pcol(l, C_GFFN + i), R, ALU.mult, ALU.mult)
                    for j in range(4):
                        P.mm(pls[j][:, 0:8], HF[:, j * 128:(j + 1) * 128], RW[:, i * 8:(i + 1) * 8],
                             start=(i == 0), stop=(i == KC - 1))
                for j in range(4):
                    P.copy(LG[:, tb * 4 + j, :], pls[j][:, 0:8])
            bc = lambda v: V(v.ap.unsqueeze(2).to_broadcast([128, 16, 8]), v.bufs)
            M1 = G1[:, 0:16]; M2 = G1[:, 16:32]
            P.reduce_max(M1, LG)
            P.tt(E1, LG, bc(M1), ALU.is_equal)
            P.stt(M8, E1, -1e30, LG, ALU.mult, ALU.add)
            P.reduce_max(M2, M8)
            P.tt(M8, M8, bc(M2), ALU.is_equal)
            GG = uf32(2240, 64)
            P.tt(GG[:, 0:16], M2, M1, ALU.subtract)
            P.act(GG[:, 16:32], GG[:, 0:16], AF.Exp)
            P.act(GG[:, 0:16], GG[:, 16:32], AF.Identity, bias=1.0)
            P.recip(GG[:, 16:32], GG[:, 0:16])
            P.act(GG[:, 0:16], GG[:, 16:32], AF.Identity, bias=1.0, scale=-1.0)
            P.tt(CB, E1, bc(GG[:, 16:32]), ALU.mult)
            P.tt(M8, M8, bc(GG[:, 0:16]), ALU.mult)
            P.tt(CB, CB, M8, ALU.add)
        ngroups = (nff + 3) // 4
        gi = 0
        pending_w2 = [None]
        for e in experts:
            if e is not None:
                cbv = COMB[e % 2]
                for tt_ in range(16):
                    if tt_ % 4 == 0: pc = rot()
                    P.mm(pc[:, (tt_ % 4) * 128:(tt_ % 4 + 1) * 128],
                         V(CB.ap[:, tt_, e:e + 1].to_broadcast([128, 128]), CB.bufs),
                         CT[:, K_ID:K_ID + 128])
                    if tt_ % 4 == 3:
                        P.act(cbv[:, (tt_ // 4) * 512:(tt_ // 4 + 1) * 512], pc, AF.Copy)
            for g in range(ngroups):
                js = list(range(g * 4, min(nff, g * 4 + 4)))
                ab = ACTB[gi % 2]; gi += 1
                for jj, j in enumerate(js):
                    w1 = wload(("w1", l, e, j))
                    w3 = wload(("w3", l, e, j))
                    for tb in range(TB):
                        p1 = rot(); p3 = rot()
                        proj_fm(w1, [H[kc][tb] for kc in range(KC)], p1)
                        proj_fm(w3, [H[kc][tb] for kc in range(KC)], p3)
                        sl = SL[:, tb % 2, :]
                        P.act(sl, p1, AF.Silu)
                        dst = ab[jj][:, tb * 512:(tb + 1) * 512]
                        if e is None:
                            P.tt(dst, p3, sl, ALU.mult)
                        else:
                            k = cnt["lt"] % 2; cnt["lt"] += 1
                            P.tt(LT[k], p3, sl, ALU.mult)
                            P.tt(dst, LT[k], cbv[:, tb * 512:(tb + 1) * 512], ALU.mult)
                def w2_emit(js=js, ab=ab, e=e, l=l):
                    w2s = [wload(("w2", l, e, j)) for j in js]
                    for i in range(KC):
                        for tb in range(TB):
                            py = PS[(i * TB + tb) % 4]
                            for jj in range(len(js)):
                                P.mm(py, w2s[jj][:, i * 128:(i + 1) * 128], ab[jj][:, tb * 512:(tb + 1) * 512],
                                     start=(jj == 0), stop=(jj == len(js) - 1))
                            P.tt(X[i][tb], py, X[i][tb], ALU.add)
                if pending_w2[0] is not None: pending_w2[0]()
                pending_w2[0] = w2_emit
        if pending_w2[0] is not None:
            pending_w2[0](); pending_w2[0] = None

    if dump is not None:
        dump(P, dict(X=X, H=H, O=O), dump_out)
    for i in range(KC):
        for tb in range(TB):
            final_ops.append(P.dma("sp", outT[i * 128:(i + 1) * 128, tb * 512:(tb + 1) * 512], X[i][tb]))
    holder["w"] = nc.dram_tensor("wbig", [max(1, len(unit_keys)), 128, 1024], F32, kind="ExternalInput").ap()
    P.emit(final_ops)
    es.close()
    return nc, unit_keys


def _blk_cols(W, j):
    K = W.shape[0]
    kc = K // 128
    return W[:, j * 128:(j + 1) * 128].reshape(kc, 128, 128).transpose(1, 0, 2).reshape(128, kc * 128)


def get_unit(key, inp):
    kind = key[0]; l = key[1]
    if kind == "win": return _blk_cols(inp["w_in"][l], key[2])
    if kind == "gate": return _blk_cols(inp["w_gate"][l], key[2])
    if kind == "memkv": return _blk_cols(inp["w_mem_kv"][l], key[2])
    if kind == "brda": return _blk_cols(inp["w_br_da"][l], key[2])
    if kind == "wout": return _blk_cols(inp["w_out"][l], key[2])
    if kind == "brpm":
        return np.concatenate([_blk_cols(inp["w_br_pool"][l], key[2]), _blk_cols(inp["w_br_mem"][l], key[2])], axis=1)
    if kind == "poolw":
        u = np.zeros((128, 1024), np.float32)
        u[:, 0:512] = inp["pool_w"][l].transpose(1, 0, 2).reshape(128, 512)
        return u
    e, j = key[2], key[3]
    if kind in ("w1", "w3"):
        if e is None: W = inp["ffn_" + kind][l // 2]
        else: W = inp["moe_" + kind][l // 2][e]
        return _blk_cols(W, j)
    if kind == "w2":
        if e is None: W = inp["ffn_w2"][l // 2]
        else: W = inp["moe_w2"][l // 2][e]
        return W[j * 128:(j + 1) * 128, :]
    raise KeyError(key)


def make_params(inp):
    PRM = np.zeros((128, DEPTH * NPC), np.float32)
    fm = lambda v: np.asarray(v, np.float32).reshape(-1, 128).T
    for l in range(DEPTH):
        b = l * NPC
        PRM[:, b + 0:b + 8] = fm(inp["norm_mix"][l])
        PRM[:, b + 8:b + 16] = fm(inp["norm_mem"][l])
        PRM[:, b + 16:b + 24] = fm(inp["norm_ffn"][l])
        PRM[:, b + 24:b + 48] = fm(inp["b_gate"][l])
        PRM[:, b + 48] = np.asarray(inp["da_q_norm"][l]).reshape(128)
        PRM[:, b + 49] = np.asarray(inp["da_k_norm"][l]).reshape(128)
        PRM[:, b + 50] = np.asarray(inp["da_out_norm"][l]).reshape(128)
        PRM[:, b + 51:b + 55] = fm(inp["pool_scale"][l])
        PRM[:, b + 55] = np.asarray(inp["mem_q_norm"][l]).reshape(128)
        PRM[:, b + 56] = np.asarray(inp["mem_k_norm"][l]).reshape(128)
        for j, nm in enumerate(["da_lam_q1", "da_lam_k1", "da_lam_q2", "da_lam_k2"]):
            PRM[:, b + 57 + 64 * j:b + 57 + 64 * (j + 1)] = np.broadcast_to(np.asarray(inp[nm][l]).reshape(1, 64), (128, 64))
    return PRM


def make_consts():
    C = np.zeros((128, NCC), np.float32)
    sl = alibi_slopes()
    kp = np.arange(128, dtype=np.float64)
    for h in range(NH):
        for dd in range(-3, 16):
            C[:, h * 20 + dd + 3] = sl[h] * (kp - 128.0 * dd - GROUPW[h] / 2.0)
        qp = np.arange(128, dtype=np.float64)[None, :]
        kk = kp[:, None]
        allowed = (kk // 64) <= (qp // 64)
        B = -sl[h] * np.abs(qp - kk) + sl[h] * (qp - 64.0)
        C[:, 160 + h * 128:160 + (h + 1) * 128] = np.where(allowed, B, -30000.0)
    C[:, 160 + 1024:160 + 1024 + 16] = (1.0 / (np.arange(16) + 1.0))[None, :]
    C[:, 160 + 1024 + 16:] = np.eye(128)
    return C


_CACHE = {}


def _get_program():
    if "p" not in _CACHE:
        _CACHE["p"] = build_program()
    return _CACHE["p"]


def pack_weights(unit_keys, inp):
    wb = np.empty((len(unit_keys), 128, 1024), np.float32)
    for n, k in enumerate(unit_keys):
        wb[n] = get_unit(k, inp)
    return wb


def kernel(**inputs):
    inp = {k: np.asarray(v) for k, v in inputs.items()}
    nc, unit_keys = _get_program()
    wb = pack_weights(unit_keys, inp)
    PRM = make_params(inp)
    CST = make_consts()
    rt = np.ascontiguousarray(inp["moe_router"][0].reshape(KC, 128, NE).transpose(1, 0, 2).reshape(128, KC * NE))
    in_maps = []
    for b in range(8):
        in_maps.append({
            "xT": np.ascontiguousarray(inp["x"][b].T),
            "memT": np.ascontiguousarray(inp["mem"][b].T),
            "params": PRM, "consts": CST, "router": rt, "wbig": wb,
        })
    res = run_bass_kernel_spmd(nc, in_maps, core_ids=list(range(8)))
    out = np.stack([np.asarray(r["outT"]).T for r in res.results], axis=0)
    return out.astype(np.float32)
```
